# Optimizing a Trainium2 kernel written in Bass

```python
import jax, jax.numpy as jnp
from jax import lax
import numpy as np


D_MODEL = 2048
BATCH = 4
SEQ = 8192
DEPTH = 4

EPS = 1e-6
CHUNK = 128
GMLP_WIDTH = D_MODEL // 4
GMLP_GROUPS = 4
GMLP_GROUP_DIM = GMLP_WIDTH // GMLP_GROUPS
ATTN_WIDTH = D_MODEL // 2
HEAD_DIM = 64
N_Q_HEADS = ATTN_WIDTH // HEAD_DIM
N_KV_HEADS = max(1, N_Q_HEADS // 8)
GQA_GROUP = N_Q_HEADS // N_KV_HEADS
WINDOW = 128
ROT_DIM = HEAD_DIM // 4
ROPE_THETA = 500000.0
MLSTM_WIDTH = D_MODEL - GMLP_WIDTH - ATTN_WIDTH
MLSTM_HEADS = 4
MLSTM_HEAD_DIM = MLSTM_WIDTH // MLSTM_HEADS
MLSTM_CHUNK = 128
CONV_WIDTH = 4
FORGET_BIAS = 3.0
D_FF = ((8 * D_MODEL + 3 * 256 - 1) // (3 * 256)) * 256
PROJ_SPLITS = (GMLP_WIDTH, GMLP_WIDTH, ATTN_WIDTH, N_KV_HEADS * HEAD_DIM, N_KV_HEADS * HEAD_DIM,
               2 * MLSTM_WIDTH, MLSTM_WIDTH, MLSTM_WIDTH, MLSTM_HEADS, MLSTM_HEADS)
IN_PROJ_WIDTH = sum(PROJ_SPLITS)

kernel_name = 'hybrid_parallel_heads_gmlp_swa_mlstm'


def rmsnorm(x, g):
    xf = x.astype(jnp.float32)
    y = xf * lax.rsqrt(jnp.mean(xf * xf, axis=-1, keepdims=True) + EPS)
    return (y * g.astype(jnp.float32)).astype(x.dtype)


def rotary_tables(seq_len):
    inv_freq = ROPE_THETA ** (-jnp.arange(0, ROT_DIM, 2, dtype=jnp.float32) / ROT_DIM)
    ang = jnp.arange(seq_len, dtype=jnp.float32)[:, None] * inv_freq[None, :]
    return jnp.cos(ang), jnp.sin(ang)


def partial_rotary(x, cos, sin):
    half = ROT_DIM // 2
    xr, xp = x[..., :ROT_DIM], x[..., ROT_DIM:]
    x1, x2 = xr[..., :half], xr[..., half:]
    c = cos[None, :, None, :].astype(x.dtype)
    s = sin[None, :, None, :].astype(x.dtype)
    return jnp.concatenate([x1 * c - x2 * s, x2 * c + x1 * s, xp], axis=-1)


def chunked_spatial_gating(u, v, v_gain, w_s, b_s):
    b, s, _ = v.shape
    nc = s // CHUNK
    u = jax.nn.gelu(u)
    v = rmsnorm(jax.nn.gelu(v), v_gain)
    vc = v.reshape(b, nc, CHUNK, GMLP_GROUPS, GMLP_GROUP_DIM)
    tril = jnp.tril(jnp.ones((CHUNK, CHUNK), dtype=bool))
    w = jnp.where(tril[None], w_s, jnp.zeros_like(w_s))
    mixed = jnp.einsum('gts,bnsgc->bntgc', w, vc) + b_s.T[None, None, :, :, None]
    return u * mixed.reshape(b, s, GMLP_WIDTH)


def sliding_window_attention(q, k, v, sinks):
    b, s, _, _ = q.shape
    blk = WINDOW
    nb = s // blk
    qb = q.reshape(b, nb, blk, N_KV_HEADS, GQA_GROUP, HEAD_DIM)
    kb = k.reshape(b, nb, blk, N_KV_HEADS, HEAD_DIM)
    vb = v.reshape(b, nb, blk, N_KV_HEADS, HEAD_DIM)
    prev = lambda t: jnp.concatenate([jnp.zeros_like(t[:, :1]), t[:, :-1]], axis=1)
    kk = jnp.concatenate([prev(kb), kb], axis=2)
    vv = jnp.concatenate([prev(vb), vb], axis=2)
    scores = jnp.einsum('bnqhgd,bnkhd->bnhgqk', qb, kk,
                        preferred_element_type=jnp.float32) * (HEAD_DIM ** -0.5)
    qi = jnp.arange(blk)[:, None]
    kj = jnp.arange(2 * blk)[None, :]
    band = (kj > qi) & (kj <= qi + blk)
    first_ok = (jnp.arange(nb)[:, None, None] > 0) | (kj >= blk)[None]
    mask = band[None] & first_ok
    scores = jnp.where(mask[None, :, None, None], scores, -jnp.inf)
    sink = sinks.astype(jnp.float32).reshape(N_KV_HEADS, GQA_GROUP)[None, None, :, :, None, None]
    mx = jnp.maximum(scores.max(axis=-1, keepdims=True), sink)
    p = jnp.exp(scores - mx)
    p = p / (p.sum(axis=-1, keepdims=True) + jnp.exp(sink - mx))
    o = jnp.einsum('bnhgqk,bnkhd->bnqhgd', p.astype(v.dtype), vv)
    return o.reshape(b, s, N_Q_HEADS * HEAD_DIM)


def causal_short_conv(x, w):
    s = x.shape[1]
    xp = jnp.pad(x, ((0, 0), (CONV_WIDTH - 1, 0), (0, 0)))
    out = xp[:, 0:s] * w[0]
    for j in range(1, CONV_WIDTH):
        out = out + xp[:, j:j + s] * w[j]
    return out


def mlstm(q, k, v, o_pre, i_pre, f_pre, i_bias, f_bias, head_gain):
    b, s, _ = q.shape
    L = MLSTM_CHUNK
    nc = s // L
    H, D = MLSTM_HEADS, MLSTM_HEAD_DIM

    def heads(t):
        return t.reshape(b, nc, L, H, D).transpose(1, 0, 3, 2, 4).astype(jnp.float32)

    def gates(t):
        return t.reshape(b, nc, L, H).transpose(1, 0, 3, 2)

    qh = heads(q) * (D ** -0.5)
    kh = heads(k)
    vh = heads(v)
    ig = gates((i_pre + i_bias).astype(jnp.float32))
    lf = gates(jax.nn.log_sigmoid((f_pre + f_bias).astype(jnp.float32)))
    causal = jnp.tril(jnp.ones((L, L), dtype=bool))

    def body(carry, inp):
        C, n, m = carry
        qc, kc, vc, ic, fc = inp
        bcum = jnp.cumsum(fc, axis=-1)
        logd = bcum[..., :, None] - bcum[..., None, :] + ic[..., None, :]
        logd = jnp.where(causal, logd, -jnp.inf)
        inter = bcum + m[..., None]
        m_t = jnp.maximum(inter, logd.max(axis=-1))
        sc = jnp.einsum('bhtd,bhsd->bhts', qc, kc) * jnp.exp(logd - m_t[..., None])
        w_inter = jnp.exp(inter - m_t)
        num = jnp.einsum('bhts,bhsd->bhtd', sc, vc) + w_inter[..., None] * jnp.einsum('bhtk,bhkv->bhtv', qc, C)
        den = sc.sum(axis=-1) + w_inter * jnp.einsum('bhtk,bhk->bht', qc, n)
        h = num / jnp.maximum(jnp.abs(den), jnp.exp(-m_t))[..., None]
        b_last = bcum[..., -1]
        logw = b_last[..., None] - bcum + ic
        m_new = jnp.maximum(b_last + m, logw.max(axis=-1))
        wk = jnp.exp(logw - m_new[..., None])
        decay = jnp.exp(b_last + m - m_new)
        C_new = decay[..., None, None] * C + jnp.einsum('bhs,bhsk,bhsv->bhkv', wk, kc, vc)
        n_new = decay[..., None] * n + jnp.einsum('bhs,bhsk->bhk', wk, kc)
        return (C_new, n_new, m_new), h

    init = (jnp.zeros((b, H, D, D), jnp.float32), jnp.zeros((b, H, D), jnp.float32),
            jnp.zeros((b, H), jnp.float32))
    _, hs = lax.scan(body, init, (qh, kh, vh, ig, lf))
    hs = hs.transpose(1, 0, 3, 2, 4).reshape(b, s, H, D)
    hs = rmsnorm(hs, head_gain.reshape(H, D)).reshape(b, s, MLSTM_WIDTH).astype(q.dtype)
    return jax.nn.sigmoid(o_pre) * hs


def hybrid_layer(x, cos, sin, norm1_g, w_in, gmlp_v_gain, gmlp_w_s, gmlp_b_s, attn_sinks,
                 mlstm_conv_w, mlstm_i_bias, mlstm_f_bias, mlstm_head_gain, w_out,
                 norm2_g, w_gate, w_up, w_down):
    b, s, _ = x.shape
    hn = rmsnorm(x, norm1_g)
    proj = hn @ w_in
    split_idx = np.cumsum(PROJ_SPLITS)[:-1].tolist()
    a_u, a_v, b_q, b_k, b_v, c_qk, c_v, c_o, c_i, c_f = jnp.split(proj, split_idx, axis=-1)
    mix_a = chunked_spatial_gating(a_u, a_v, gmlp_v_gain, gmlp_w_s, gmlp_b_s)
    q = partial_rotary(b_q.reshape(b, s, N_Q_HEADS, HEAD_DIM), cos, sin)
    k = partial_rotary(b_k.reshape(b, s, N_KV_HEADS, HEAD_DIM), cos, sin)
    v = b_v.reshape(b, s, N_KV_HEADS, HEAD_DIM)
    mix_b = sliding_window_attention(q, k, v, attn_sinks)
    c_qk = jax.nn.silu(causal_short_conv(c_qk, mlstm_conv_w))
    c_q, c_k = jnp.split(c_qk, 2, axis=-1)
    mix_c = mlstm(c_q, c_k, c_v, c_o, c_i, c_f, mlstm_i_bias, mlstm_f_bias, mlstm_head_gain)
    mixed = jnp.concatenate([mix_a, mix_b, mix_c], axis=-1)
    h = x + mixed @ w_out
    hn2 = rmsnorm(h, norm2_g)
    return h + (jax.nn.silu(hn2 @ w_gate) * (hn2 @ w_up)) @ w_down


def setup_inputs(seed: int = 0) -> dict:
    key = jax.random.key(seed)
    ks = jax.random.split(key, 17)
    f32 = jnp.float32
    nrm = lambda k, shape, scale: jax.random.normal(k, shape, f32) * scale
    return {
        'x': nrm(ks[0], (BATCH, SEQ, D_MODEL), 1.0),
        'norm1_g': 1.0 + nrm(ks[1], (DEPTH, D_MODEL), 0.02),
        'w_in': nrm(ks[2], (DEPTH, D_MODEL, IN_PROJ_WIDTH), D_MODEL ** -0.5),
        'gmlp_v_gain': 1.0 + nrm(ks[3], (DEPTH, GMLP_WIDTH), 0.02),
        'gmlp_w_s': nrm(ks[4], (DEPTH, GMLP_GROUPS, CHUNK, CHUNK), CHUNK ** -0.5),
        'gmlp_b_s': 1.0 + nrm(ks[5], (DEPTH, GMLP_GROUPS, CHUNK), 0.02),
        'attn_sinks': nrm(ks[6], (DEPTH, N_Q_HEADS), 0.5),
        'mlstm_conv_w': nrm(ks[7], (DEPTH, CONV_WIDTH, 2 * MLSTM_WIDTH), CONV_WIDTH ** -0.5),
        'mlstm_i_bias': nrm(ks[8], (DEPTH, MLSTM_HEADS), 0.1),
        'mlstm_f_bias': FORGET_BIAS + nrm(ks[9], (DEPTH, MLSTM_HEADS), 0.1),
        'mlstm_head_gain': 1.0 + nrm(ks[10], (DEPTH, MLSTM_WIDTH), 0.02),
        'w_out': nrm(ks[11], (DEPTH, D_MODEL, D_MODEL), D_MODEL ** -0.5),
        'norm2_g': 1.0 + nrm(ks[12], (DEPTH, D_MODEL), 0.02),
        'w_gate': nrm(ks[13], (DEPTH, D_MODEL, D_FF), D_MODEL ** -0.5),
        'w_up': nrm(ks[14], (DEPTH, D_MODEL, D_FF), D_MODEL ** -0.5),
        'w_down': nrm(ks[15], (DEPTH, D_FF, D_MODEL), D_FF ** -0.5),
        'final_g': 1.0 + nrm(ks[16], (D_MODEL,), 0.02),
    }


def reference(x, norm1_g, w_in, gmlp_v_gain, gmlp_w_s, gmlp_b_s, attn_sinks, mlstm_conv_w,
              mlstm_i_bias, mlstm_f_bias, mlstm_head_gain, w_out, norm2_g, w_gate, w_up,
              w_down, final_g):
    cos, sin = rotary_tables(x.shape[1])
    h = x
    for l in range(DEPTH):
        h = hybrid_layer(h, cos, sin, norm1_g[l], w_in[l], gmlp_v_gain[l], gmlp_w_s[l], gmlp_b_s[l],
                         attn_sinks[l], mlstm_conv_w[l], mlstm_i_bias[l], mlstm_f_bias[l],
                         mlstm_head_gain[l], w_out[l], norm2_g[l], w_gate[l], w_up[l], w_down[l])
    return rmsnorm(h, final_g)
```

```python
import numpy as np
from contextlib import ExitStack
import concourse.bass as bass
import concourse.mybir as mybir
from concourse.bass_utils import run_bass_kernel_spmd

F32 = mybir.dt.float32
BF16 = mybir.dt.bfloat16
AF = mybir.ActivationFunctionType
ALU = mybir.AluOpType

D = 2048
KD = 16
T = 512
NCH = 4
DFF = 5632
KF = 44
SEQ = 8192
DEPTH = 4
EPS = 1e-6
UW = 2048
NS = 6
C_AU, C_AV, C_BQ, C_BK, C_BV, C_CQK, C_CV, C_CO, C_CI, C_CF = 0, 512, 1024, 2048, 2176, 2304, 3328, 3840, 4352, 4356
UNITS_PER_LAYER = 4 + 8 + 2 + 2 + 8 + 14 + 16 + 88 + 64


def _unit_cols(w, cols):
    K = w.shape[0]
    sub = w[:, cols]
    sub = sub.reshape(K // 128, 128, len(cols))
    return np.ascontiguousarray(sub.transpose(1, 0, 2)).reshape(128, -1)


def _pad_unit(u):
    out = np.zeros((128, UW), np.float32)
    out[:, : u.shape[1]] = u
    return out


def _layer_units(w_in, conv_w, w_out, w_gate, w_up, w_down):
    units = []
    ar = np.arange
    for i in range(4):
        units.append(_unit_cols(w_in, C_AU + i * 128 + ar(128)))
    for i in range(8):
        units.append(_unit_cols(w_in, C_BQ + i * 128 + ar(128)))
    for kv in range(2):
        c = C_BK + kv * 64 + ar(64)
        units.append(_unit_cols(w_in, np.concatenate([c, c])))
    for k in range(2):
        u = np.zeros((128, 16, 128), np.float32)
        for ml in range(16):
            m = k * 16 + ml
            ch, tap = m // 4, m % 4
            u[ar(128), ml, ar(128)] = conv_w[tap, ch * 128 + ar(128)]
        units.append(u.reshape(128, -1))
    for i in range(8):
        units.append(_unit_cols(w_in, C_CQK + i * 128 + ar(128)))
    for base in (C_AV, C_CV, C_CO):
        for u4 in range(4):
            rows = w_in[u4 * 512:(u4 + 1) * 512]
            units.append(_unit_cols(rows, base + ar(512)))
    cols = np.concatenate([C_BV + ar(128), C_CI + ar(4), C_CF + ar(4)])
    for u2 in range(2):
        rows = w_in[u2 * 1024:(u2 + 1) * 1024]
        units.append(_pad_unit(_unit_cols(rows, cols)))
    for o in range(16):
        units.append(_unit_cols(w_out, o * 128 + ar(128)))
    for f in range(44):
        units.append(_unit_cols(w_gate, f * 128 + ar(128)))
        units.append(_unit_cols(w_up, f * 128 + ar(128)))
    for o in range(16):
        for q in range(4):
            rows = w_down[q * 1408:(q + 1) * 1408]
            units.append(_pad_unit(_unit_cols(rows, o * 128 + ar(128))))
    assert len(units) == UNITS_PER_LAYER
    return np.stack(units, 0)


def _rope_tables(seq):
    inv_freq = (500000.0 ** (-np.arange(0, 16, 2, dtype=np.float32) / np.float32(16))).astype(np.float32)
    ang = (np.arange(seq, dtype=np.float32)[:, None] * inv_freq[None, :]).astype(np.float32)
    cos, sin = np.cos(ang).astype(np.float32), np.sin(ang).astype(np.float32)
    C = np.ones((128, seq), np.float32)
    S = np.zeros((128, seq), np.float32)
    for hh in range(2):
        for i in range(8):
            C[hh * 64 + i] = cos[:, i]
            C[hh * 64 + 8 + i] = cos[:, i]
            S[hh * 64 + i] = sin[:, i]
            S[hh * 64 + 8 + i] = sin[:, i]
    return C, S


def _consts():
    ar = np.arange(128)
    ident = np.eye(128, dtype=np.float32)
    mle = (ar[:, None] <= ar[None, :]).astype(np.float32)
    mgt = (ar[:, None] > ar[None, :]).astype(np.float32)
    ones = np.ones((128, 128), np.float32)
    RT = np.zeros((128, 128), np.float32)
    for hh in range(2):
        for i in range(8):
            RT[hh * 64 + 8 + i, hh * 64 + i] = -1.0
            RT[hh * 64 + i, hh * 64 + 8 + i] = 1.0
    return np.stack([ident, mle, mgt, ones, RT], 1)


class Buf:
    __slots__ = ("w", "r", "excl")

    def __init__(self, excl=False):
        self.w = {}
        self.r = {}
        self.excl = excl


def _merge(d, s):
    for k, v in s.items():
        if d.get(k, 0) < v:
            d[k] = v


def I(name, *args, **kw):
    return (name, args, kw)


class Sched:
    ENG = ("pe", "act", "dve", "pool", "sp")

    def __init__(self):
        self.ops = {e: [] for e in self.ENG}
        self.cnt = {}
        self.semh = {}
        self.seen = {e: {} for e in self.ENG}

    def add_sem(self, key, handle):
        self.semh[key] = handle
        self.cnt[key] = 0

    def _wait(self, eng, reads, writes, extra=None):
        deps = {}
        for b in reads:
            _merge(deps, b.w)
            if b.excl:
                _merge(deps, b.r)
        for b in writes:
            _merge(deps, b.w)
            _merge(deps, b.r)
        if extra:
            _merge(deps, extra)
        seen = self.seen[eng]
        for k, v in deps.items():
            if eng == "pe" and k == "pe":
                continue
            if seen.get(k, 0) < v:
                seen[k] = v
                self.ops[eng].append(("wait", (self.semh[k], v)))

    def _commit(self, k, v, reads, writes):
        for b in reads:
            if b.excl:
                b.w = {k: v}
                b.r = {}
            elif b.r.get(k, 0) < v:
                b.r[k] = v
        for b in writes:
            b.w = {k: v}
            b.r = {}

    def op(self, eng, ins, reads=(), writes=()):
        self.group(eng, [ins], reads, writes)

    def group(self, eng, inss, reads=(), writes=()):
        self._wait(eng, reads, writes)
        for ins in inss[:-1]:
            self.ops[eng].append(("ins", (ins, None, 0)))
        self.cnt[eng] += 1
        self.ops[eng].append(("ins", (inss[-1], self.semh[eng], 1)))
        self._commit(eng, self.cnt[eng], reads, writes)

    def dma(self, queue, ins, semkey, reads=(), writes=(), track=True):
        self._wait(queue, reads, writes if track else ())
        self.cnt[semkey] += 16
        self.ops[queue].append(("ins", (ins, self.semh[semkey], 16)))
        if track:
            self._commit(semkey, self.cnt[semkey], reads, writes)

    def final_wait(self, eng, bufs):
        self._wait(eng, (), bufs)

    def replay(self, eng, e):
        for kind, p in self.ops[eng]:
            if kind == "wait":
                e.wait_ge(p[0], p[1])
            else:
                (name, args, kw), sem, inc = p
                r = getattr(e, name)(*args, **kw)
                if sem is not None:
                    r.then_inc(sem, inc)


def build_program(L, NT, final_norm=True, dbg=0):
    nc = bass.Bass("TRN2", target_bir_lowering=False)
    NU = L * UNITS_PER_LAYER
    S = NT * T
    BCW = 1568
    dt = nc.dram_tensor
    x_d = dt("x", [NT, 128, KD * T], F32, kind="ExternalInput").ap()
    w32_d = dt("w32", [NU, 128, UW], F32, kind="ExternalInput").ap()
    cos_d = dt("cosT", [128, S], F32, kind="ExternalInput").ap()
    sin_d = dt("sinT", [128, S], F32, kind="ExternalInput").ap()
    cst_d = dt("consts", [128, 5 * 128], F32, kind="ExternalInput").ap()
    g12_d = dt("g12", [128, L * 2 * KD + KD], F32, kind="ExternalInput").ap()
    wst_d = dt("wsT", [128, L * 4 * 128], F32, kind="ExternalInput").ap()
    bc_d = dt("bcast", [1, L * BCW], F32, kind="ExternalInput").ap()
    y_d = dt("y", [NT, 128, KD * T], F32, kind="ExternalOutput").ap()
    wbf_l = [dt("wbf%d" % l, [UNITS_PER_LAYER, 128, UW], BF16, kind="Internal").ap() for l in range(L)]
    wbf_u = lambda k: wbf_l[k // UNITS_PER_LAYER][k % UNITS_PER_LAYER]
    dbg_d = dt("dbg_a", [128, KD * T], BF16, kind="ExternalOutput").ap() if dbg else None

    sc = Sched()
    es = ExitStack()
    with es:
        def sb(name, shape, dtype):
            return es.enter_context(nc.sbuf_tensor("s_" + name, shape, dtype))

        for k in Sched.ENG:
            sc.add_sem(k, es.enter_context(nc.semaphore("sem_" + k)))
        for k in ["cvt", "xld", "cs", "yst", "setup", "wst"] + ["slot%d" % i for i in range(NS)]:
            sc.add_sem(k, es.enter_context(nc.semaphore("sem_" + k)))

        xT = sb("xT", [128, KD, T], F32)
        aT = sb("aT", [128, KD, T], BF16)
        big = sb("big", [128, KF * T], BF16)
        hT = big[:, :].rearrange("p (f t) -> p f t", t=T)
        yv = big[:, 0:KD * T * 2].bitcast(F32)
        yv3 = yv.rearrange("p (c t) -> p c t", t=T)
        off = [0]

        def carve(n):
            a = off[0]
            off[0] += n
            assert off[0] <= KF * T
            return big[:, a:a + n]
        uT = carve(4 * T).rearrange("p (c t) -> p c t", t=T)
        qT = carve(8 * T).rearrange("p (c t) -> p c t", t=T)
        kTd = carve(2 * 640).rearrange("p (c t) -> p c t", t=640)
        cx = carve(8 * 516).rearrange("p (c t) -> p c t", t=516)
        mqk = carve(8 * T).rearrange("p (c t) -> p c t", t=T)
        vg = carve(4 * T).rearrange("p (c t) -> p c t", t=T)
        mv = carve(4 * 4 * 130).rearrange("p (c h d) -> p c h d", h=4, d=130)
        sgo = carve(4 * T).rearrange("p (c t) -> p c t", t=T)
        av = carve(5 * 2 * 66).rearrange("p (c h d) -> p c h d", h=2, d=66)
        wsl = sb("wsl", [128, NS, UW], BF16)
        cst32 = sb("cst32", [128, 5, 128], F32)
        cstbf = sb("cstbf", [128, 5, 128], BF16)
        g12 = sb("g12", [128, L * 2 * KD + KD], F32)
        wstm = sb("wstm", [128, L * 4, 128], BF16)
        bcs = sb("bcs", [128, L * BCW], F32)
        esink = sb("esink", [128, L, 16], F32)
        cs_t = sb("cs_t", [128, 2, T], F32)
        gts = sb("gts", [128, NCH, 8], F32)
        epsT = sb("epsT", [128, 2], F32)
        cxh = sb("cxh", [128, L, 8, 4], BF16)
        avh = sb("avh", [128, L, 2, 66], BF16)
        kdh = sb("kdh", [128, L, 2, 128], BF16)
        C32 = sb("C32", [128, L, 4, 130], F32)
        Cbf = sb("Cbf", [128, L, 4, 130], BF16)
        sq = sb("sq", [128, 2, T], BF16)
        rs_b = sb("rs_b", [128, T], F32)
        xq = sb("xq", [128, 2, T], BF16)
        t1 = sb("t1", [128, T], F32)
        ex = sb("ex", [128, 2, T], BF16)
        pT = sb("pT", [128, 4, T], BF16)
        gv = sb("gv", [128, 2, T], F32)
        junk = sb("junk", [128, T], F32)
        mixb = sb("mixb", [128, 16, 64], BF16)
        mixc = sb("mixc", [128, 4, 128], BF16)
        mixf = sb("mixf", [128, 4, 128], F32)
        Sm = sb("Sm", [128, 4, 128], BF16)
        ktil = sb("ktil", [128, 4, 128], BF16)
        gtmp = sb("gtmp", [128, T], F32)
        t2, rs_a, sg = gtmp, junk, xq
        cdiag = sb("cdiag", [128, 32, 128], BF16)
        sm_ = sb("small", [128, 24, 16], F32)
        psb = [es.enter_context(nc.psum_tensor("ps%d" % i, [128, 512], F32)) for i in range(8)]

        B = Buf
        BL = lambda n: [Buf() for _ in range(n)]
        b_x, b_a, b_h, b_y = BL(KD), BL(KD), BL(KF), B()
        b_uT, b_qT, b_kTd, b_cx, b_mqk = BL(4), BL(8), BL(2), BL(8), BL(8)
        b_vg, b_mv, b_sgo, b_av, b_gts = BL(4), BL(4), BL(4), BL(5), B()
        alias_small = b_uT + b_qT + b_kTd + b_cx + b_mqk + b_vg + b_mv + b_sgo + b_av
        b_slot = BL(NS)
        b_wbf = B()
        b_cst, b_g12, b_wst32, b_wstm, b_bcs, b_esink, b_eps, b_cs = B(), B(), B(), B(), B(), B(), B(), B()
        b_cxh, b_avh, b_kdh, b_C32, b_Cbf = BL(L), BL(L), BL(L), BL(L), BL(L)
        b_sq = BL(2)
        b_rsb, b_t1, b_junk, b_gtmp = B(), B(), B(), B()
        b_rsa, b_t2 = b_junk, b_gtmp
        b_xq, b_ex, b_gv, b_pT = BL(2), BL(2), BL(2), BL(4)
        b_sg = b_xq
        b_mixb, b_mixc, b_mixf, b_Sm, b_ktil, b_sm = B(), B(), B(), B(), B(), B()
        b_ps = [Buf(excl=True) for _ in range(8)]
        b_cdiag = B()
        rr = {"ps": 0, "sq": 0, "xq": 0, "ex": 0, "gv": 0, "sg": 0, "pT": 0}

        def nxt(key, n):
            i = rr[key]
            rr[key] = (i + 1) % n
            return i

        def bank():
            i = nxt("ps", 8)
            return psb[i], b_ps[i]

        ident_bf, mle_bf, mgt_bf, ones_bf, RT_bf = [cstbf[:, i, :] for i in range(5)]
        mle_32, ones_32 = cst32[:, 1, :], cst32[:, 3, :]
        eps_ap, one_ap = epsT[:, 0:1], epsT[:, 1:2]
        BC = lambda l, a, b: bcs[:, l * BCW + a:l * BCW + b]

        wq = {"issued": 0, "m": 0}
        total_units = NT * NU

        def take(n=1):
            m = wq["m"]
            lim = min(total_units, m + NS)
            while wq["issued"] < lim:
                k = wq["issued"]
                s = k % NS
                sc.dma("sp", I("dma_start", out=wsl[:, s, :], in_=wbf_u(k % NU)), "slot%d" % s,
                       reads=[b_wbf], writes=[b_slot[s]])
                wq["issued"] += 1
            assert wq["issued"] >= min(total_units, m + n)
            wq["m"] += n
            return [((m + i) % NS) for i in range(n)]

        sc.dma("sp", I("dma_start", out=cst32[:, :, :], in_=cst_d.rearrange("p (a b) -> p a b", b=128)), "setup", track=False)
        sc.dma("sp", I("dma_start", out=g12[:, :], in_=g12_d), "setup", track=False)
        sc.dma("sp", I("dma_start", out=bcs[:, :], in_=bc_d.partition_broadcast(128)), "setup", track=False)
        for b in (b_cst, b_g12, b_bcs):
            b.w = {"setup": sc.cnt["setup"]}
        for u in range(NU):
            sc.dma("pool", I("dma_start", out=wbf_u(u), in_=w32_d[u]), "cvt", track=False)
        b_wbf.w = {"cvt": sc.cnt["cvt"]}
        dv = lambda ins, r=(), w=(): sc.op("dve", ins, reads=list(r), writes=list(w))
        ac = lambda ins, r=(), w=(): sc.op("act", ins, reads=list(r), writes=list(w))
        dv(I("tensor_copy", out=cstbf[:, :, :], in_=cst32[:, :, :]), r=[b_cst], w=[b_cst])
        for l in range(L):
            sc.dma("sp", I("dma_start", out=gtmp[:, :], in_=wst_d[:, l * 512:(l + 1) * 512]), "wst", writes=[b_gtmp])
            dv(I("tensor_tensor", out=wstm[:, l * 4:(l + 1) * 4, :], in0=gtmp[:, :].rearrange("p (a b) -> p a b", b=128),
                 in1=cst32[:, 1:2, :].broadcast_to([128, 4, 128]), op=ALU.mult), r=[b_gtmp, b_cst], w=[b_wstm])
        for l in range(L):
            ac(I("activation", out=esink[:, l, :], in_=BC(l, 1536, 1552), func=AF.Exp), r=[b_bcs], w=[b_esink])
        dv(I("memset", big[:, :], 0.0), w=alias_small + b_h)
        for (tns, bl) in ((cxh, b_cxh), (avh, b_avh), (kdh, b_kdh), (C32, b_C32), (Cbf, b_Cbf)):
            dv(I("memset", tns[:], 0.0), w=bl)
        dv(I("memset", avh[:, :, :, 64:65], 1.0), w=b_avh)
        dv(I("memset", epsT[:, 0:1], EPS), w=[b_eps])
        dv(I("memset", epsT[:, 1:2], 1.0), w=[b_eps])

        def norm_stats():
            ps, bps = bank()
            for dch in range(KD):
                i = nxt("sq", 2)
                ac(I("activation", out=sq[:, i, :], in_=xT[:, dch, :], func=AF.Square), r=[b_x[dch]], w=[b_sq[i]])
                sc.group("pe", [I("matmul", ps[:, :], ones_bf, sq[:, i, :], start=(dch == 0), stop=(dch == KD - 1))],
                         reads=[b_sq[i], b_cst], writes=[bps])
            ac(I("activation", out=rs_a[:, :], in_=ps[:, :], func=AF.Sqrt, bias=eps_ap, scale=1.0 / D), r=[bps, b_eps], w=[b_rsa])
            dv(I("reciprocal", out=rs_b[:, :], in_=rs_a[:, :]), r=[b_rsa], w=[b_rsb])

        def rmsnorm_to_aT(gcol):
            norm_stats()
            for dch in range(KD):
                dv(I("scalar_tensor_tensor", out=aT[:, dch, :], in0=xT[:, dch, :], scalar=g12[:, gcol + dch:gcol + dch + 1],
                     in1=rs_b[:, :], op0=ALU.mult, op1=ALU.mult), r=[b_x[dch], b_g12, b_rsb], w=[b_a[dch]])

        def fm_unit(slot):
            ps, bps = bank()
            sc.group("pe", [I("matmul", ps[:, :], wsl[:, slot, dch * 128:(dch + 1) * 128], aT[:, dch, :],
                              start=(dch == 0), stop=(dch == KD - 1)) for dch in range(KD)],
                     reads=b_a + [b_slot[slot]], writes=[bps])
            return ps, bps

        smb = [b_sm]
        R = lambda r, a=0, b=16: sm_[:, r, a:b]

        def layer_body(ti, l):
            first = (ti == 0)
            hdeps = {}
            for b in b_h + [b_y]:
                _merge(hdeps, b.w)
                _merge(hdeps, b.r)
            for b in alias_small:
                _merge(b.r, hdeps)
            if dbg == 2:
                return
            rmsnorm_to_aT(l * 2 * KD)
            ac(I("copy", out=cx[:, :, 0:4], in_=cxh[:, l, :, :]), r=[b_cxh[l]], w=b_cx)
            ac(I("copy", out=av[:, 0, :, :], in_=avh[:, l, :, :]), r=[b_avh[l]], w=[b_av[0]])
            ac(I("copy", out=kTd[:, :, 0:128], in_=kdh[:, l, :, :]), r=[b_kdh[l]], w=b_kTd)
            dv(I("memset", mv[:, :, :, 128:129], 1.0), w=b_mv)
            dv(I("memset", av[:, 1:5, :, 64:65], 1.0), w=b_av[1:5])
            if dbg == 3:
                return
            for i in range(4):
                (s,) = take(1)
                ps, bps = fm_unit(s)
                ac(I("activation", out=uT[:, i, :], in_=ps[:, :], func=AF.Gelu_apprx_tanh), r=[bps], w=[b_uT[i]])
            if dbg == 31:
                return
            for i in range(10):
                (s,) = take(1)
                ps, bps = fm_unit(s)
                j = nxt("xq", 2)
                ac(I("copy", out=xq[:, j, :], in_=ps[:, :]), r=[bps], w=[b_xq[j]])
                ps2, bps2 = bank()
                sc.group("pe", [I("matmul", ps2[:, :], RT_bf, xq[:, j, :], start=True, stop=True)],
                         reads=[b_xq[j], b_cst], writes=[bps2])
                dv(I("tensor_tensor", out=t1[:, :], in0=ps[:, :], in1=cs_t[:, 0, :], op=ALU.mult), r=[bps, b_cs], w=[b_t1])
                dv(I("tensor_tensor", out=t2[:, :], in0=ps2[:, :], in1=cs_t[:, 1, :], op=ALU.mult), r=[bps2, b_cs], w=[b_t2])
                if i < 8:
                    dv(I("tensor_tensor", out=qT[:, i, :], in0=t1[:, :], in1=t2[:, :], op=ALU.add), r=[b_t1, b_t2], w=[b_qT[i]])
                else:
                    dv(I("tensor_tensor", out=kTd[:, i - 8, 128:640], in0=t1[:, :], in1=t2[:, :], op=ALU.add),
                       r=[b_t1, b_t2], w=[b_kTd[i - 8]])
            if dbg == 32:
                return
            cslots = take(2)
            for k2 in range(2):
                ac(I("copy", out=cdiag[:, k2 * 16:(k2 + 1) * 16, :], in_=wsl[:, cslots[k2], :].rearrange("p (m c) -> p m c", c=128)),
                   r=[b_slot[cslots[k2]]], w=[b_cdiag])
            for i in range(8):
                (s,) = take(1)
                ps, bps = fm_unit(s)
                ac(I("copy", out=cx[:, i, 4:516], in_=ps[:, :]), r=[bps], w=[b_cx[i]])
                ps2, bps2 = bank()
                fns = []
                for tap in range(4):
                    m = i * 4 + tap
                    fns.append(I("matmul", ps2[:, :], cdiag[:, m, :], cx[:, i, 1 + tap:1 + tap + T],
                                 start=(tap == 0), stop=(tap == 3)))
                sc.group("pe", fns, reads=[b_cx[i], b_cdiag], writes=[bps2])
                ac(I("activation", out=mqk[:, i, :], in_=ps2[:, :], func=AF.Silu), r=[bps2], w=[b_mqk[i]])
            ac(I("copy", out=cxh[:, l, :, :], in_=cx[:, :, 512:516]), r=b_cx, w=[b_cxh[l]])
            ac(I("copy", out=kdh[:, l, :, :], in_=kTd[:, :, 512:640]), r=b_kTd, w=[b_kdh[l]])
            if dbg == 4:
                return
            for grp in range(3):
                slots = take(4)
                for tch in range(NCH):
                    ps, bps = bank()
                    fns = []
                    for u4 in range(4):
                        for dl in range(4):
                            dch = u4 * 4 + dl
                            fns.append(I("matmul", ps[:, :], aT[:, dch, tch * 128:(tch + 1) * 128],
                                         wsl[:, slots[u4], dl * 512:(dl + 1) * 512], start=(dch == 0), stop=(dch == KD - 1)))
                    sc.group("pe", fns, reads=b_a + [b_slot[s] for s in slots], writes=[bps])
                    if grp == 0:
                        j = nxt("gv", 2)
                        ac(I("activation", out=gv[:, j, :], in_=ps[:, :], func=AF.Gelu_apprx_tanh), r=[bps], w=[b_gv[j]])
                        ac(I("activation", out=junk[:, :], in_=gv[:, j, :], func=AF.Square, accum_out=R(0, tch, tch + 1)),
                           r=[b_gv[j]], w=[b_junk, b_sm])
                        ac(I("activation", out=R(1, tch, tch + 1), in_=R(0, tch, tch + 1), func=AF.Sqrt, bias=eps_ap, scale=1.0 / 512),
                           r=[b_sm, b_eps], w=smb)
                        dv(I("reciprocal", out=R(2, tch, tch + 1), in_=R(1, tch, tch + 1)), r=smb, w=smb)
                        dv(I("scalar_tensor_tensor", out=vg[:, tch, :], in0=gv[:, j, :], scalar=R(2, tch, tch + 1),
                             in1=BC(l, 0, 512), op0=ALU.mult, op1=ALU.mult), r=[b_gv[j], b_sm, b_bcs], w=[b_vg[tch]])
                    elif grp == 1:
                        ac(I("copy", out=mv[:, tch, :, 0:128], in_=ps[:, :].rearrange("p (h d) -> p h d", d=128)),
                           r=[bps], w=[b_mv[tch]])
                    else:
                        ac(I("activation", out=sgo[:, tch, :], in_=ps[:, :], func=AF.Sigmoid), r=[bps], w=[b_sgo[tch]])
            slots = take(2)
            for tch in range(NCH):
                ps, bps = bank()
                fns = []
                for u2 in range(2):
                    for dl in range(8):
                        dch = u2 * 8 + dl
                        fns.append(I("matmul", ps[:, 0:136], aT[:, dch, tch * 128:(tch + 1) * 128],
                                     wsl[:, slots[u2], dl * 136:(dl + 1) * 136], start=(dch == 0), stop=(dch == KD - 1)))
                sc.group("pe", fns, reads=b_a + [b_slot[s] for s in slots], writes=[bps])
                dv(I("tensor_copy", out=av[:, 1 + tch, :, 0:64], in_=ps[:, 0:128].rearrange("p (h d) -> p h d", d=64)),
                   r=[bps], w=[b_av[1 + tch]])
                dv(I("tensor_copy", out=gts[:, tch, :], in_=ps[:, 128:136]), r=[bps], w=[b_gts])
            ac(I("copy", out=avh[:, l, :, 0:64], in_=av[:, 4, :, 0:64]), r=[b_av[4]], w=[b_avh[l]])

            if dbg == 5:
                return
            G = lambda r: sm_[:, r, :].rearrange("p (c h) -> p c h", h=4)
            fb = BC(l, 1556, 1560).unsqueeze(1).broadcast_to([128, NCH, 4])
            ib = BC(l, 1552, 1556).unsqueeze(1).broadcast_to([128, NCH, 4])
            dv(I("tensor_tensor", out=G(3), in0=gts[:, :, 4:8], in1=fb, op=ALU.add), r=[b_gts, b_bcs, b_sm], w=smb)
            dv(I("tensor_tensor", out=G(4), in0=gts[:, :, 0:4], in1=ib, op=ALU.add), r=[b_gts, b_bcs, b_sm], w=smb)
            dv(I("tensor_single_scalar", out=R(5), in_=R(3), scalar=-1.0, op=ALU.mult), r=smb, w=smb)
            dv(I("tensor_tensor", out=R(5), in0=R(5), in1=R(3), op=ALU.max), r=smb, w=smb)
            dv(I("tensor_single_scalar", out=R(6), in_=R(3), scalar=0.0, op=ALU.min), r=smb, w=smb)
            ac(I("activation", out=R(7), in_=R(5), func=AF.Exp, scale=-1.0), r=smb, w=smb)
            ac(I("activation", out=R(8), in_=R(7), func=AF.Ln, bias=one_ap), r=[b_sm, b_eps], w=smb)
            dv(I("tensor_tensor", out=R(9), in0=R(6), in1=R(8), op=ALU.subtract), r=smb, w=smb)
            psg, bpsg = bank()
            sc.group("pe", [I("matmul", psg[:, 0:16], mle_32, R(9), start=True, stop=True),
                            I("matmul", psg[:, 16:32], ones_32, R(9), start=True, stop=True)],
                     reads=[b_sm, b_cst], writes=[bpsg])
            dv(I("tensor_tensor", out=R(10), in0=R(4), in1=psg[:, 0:16], op=ALU.subtract), r=[b_sm, bpsg], w=smb)
            dv(I("tensor_tensor", out=R(11), in0=R(10), in1=psg[:, 16:32], op=ALU.add), r=[b_sm, bpsg], w=smb)
            ac(I("activation", out=R(12), in_=R(10), func=AF.Exp), r=smb, w=smb)
            ac(I("activation", out=R(13), in_=R(11), func=AF.Exp), r=smb, w=smb)
            ac(I("activation", out=R(14), in_=psg[:, 0:16], func=AF.Exp), r=[bpsg, b_sm], w=smb)
            ac(I("activation", out=R(15), in_=psg[:, 16:32], func=AF.Exp), r=[bpsg, b_sm], w=smb)
            dv(I("tensor_single_scalar", out=R(14), in_=R(14), scalar=float(128 ** -0.5), op=ALU.mult), r=smb, w=smb)

            if dbg == 6:
                sc.dma("sp", I("dma_start", out=dbg_d[:, 0:4096], in_=mqk[:, :, :].rearrange("p c t -> p (c t)")), "setup", reads=b_mqk + b_h)
                sc.dma("sp", I("dma_start", out=dbg_d[:, 4096:6176], in_=mv[:, :, :, :].rearrange("p c h d -> p (c h d)")), "setup", reads=b_mv + b_h)
                return
            for c in range(NCH):
                tk = slice(c * 128, (c + 1) * 128)
                seq_first = first and c == 0
                ps, bps = bank()
                sc.group("pe", [I("matmul", ps[:, g * 128:(g + 1) * 128], vg[:, c, g * 128:(g + 1) * 128],
                                  wstm[:, l * 4 + g, :], start=True, stop=True) for g in range(4)],
                         reads=[b_vg[c], b_wstm], writes=[bps])
                dv(I("tensor_tensor", out=gtmp[:, :], in0=ps[:, :], in1=BC(l, 1024, 1536), op=ALU.add), r=[bps, b_bcs], w=[b_gtmp])
                dv(I("tensor_tensor", out=aT[:, 0:4, tk], in0=gtmp[:, :].rearrange("p (g t) -> p g t", t=128),
                     in1=uT[:, :, tk], op=ALU.mult), r=[b_gtmp] + b_uT, w=b_a[0:4])
                for kv in range(2):
                    for par in range(2):
                        blks = [c + 1] if seq_first else [c, c + 1]
                        pidx = []
                        for blk in blks:
                            ps, bps = bank()
                            sc.group("pe", [I("matmul", ps[:, :], kTd[par * 64:(par + 1) * 64, kv, blk * 128:(blk + 1) * 128],
                                              qT[par * 64:(par + 1) * 64, 4 * kv:4 * kv + 4, tk], start=True, stop=True)],
                                     reads=[b_kTd[kv]] + b_qT[4 * kv:4 * kv + 4], writes=[bps])
                            j = nxt("ex", 2)
                            ac(I("activation", out=ex[:, j, :], in_=ps[:, :], func=AF.Exp, scale=0.125), r=[bps], w=[b_ex[j]])
                            pi = nxt("pT", 4)
                            msk = (mle_bf if blk == c + 1 else mgt_bf).unsqueeze(1).broadcast_to([128, 4, 128])
                            dv(I("tensor_tensor", out=pT[:, pi, :].rearrange("p (h t) -> p h t", t=128),
                                 in0=ex[:, j, :].rearrange("p (h t) -> p h t", t=128), in1=msk, op=ALU.mult),
                               r=[b_ex[j], b_cst], w=[b_pT[pi]])
                            pidx.append((pi, blk))
                        pso, bpso = bank()
                        fns = []
                        for i in range(4):
                            for n, (pi, blk) in enumerate(pidx):
                                fns.append(I("matmul", pso[:, i * 65:(i + 1) * 65], pT[:, pi, i * 128:(i + 1) * 128],
                                             av[:, blk, kv, 0:65], start=(n == 0), stop=(n == len(pidx) - 1)))
                        sc.group("pe", fns, reads=[b_pT[p] for p, _ in pidx] + [b_av[blk] for _, blk in pidx], writes=[bpso])
                        h0 = 8 * kv + par
                        pso3 = pso[:, 0:260].rearrange("p (h d) -> p h d", d=65)
                        es4 = esink[:, l, :].rearrange("p (i two) -> p i two", two=2)[:, 4 * kv:4 * kv + 4, par]
                        mb4 = mixb[:, :, :].rearrange("p (i two) d -> p i two d", two=2)[:, 4 * kv:4 * kv + 4, par, :]
                        dv(I("tensor_tensor", out=R(16, 0, 4), in0=pso3[:, :, 64], in1=es4, op=ALU.add), r=[bpso, b_esink, b_sm], w=smb)
                        dv(I("reciprocal", out=R(17, 0, 4), in_=R(16, 0, 4)), r=smb, w=smb)
                        dv(I("tensor_tensor", out=mb4, in0=pso3[:, :, 0:64],
                             in1=R(17, 0, 4).unsqueeze(2).broadcast_to([128, 4, 64]), op=ALU.mult), r=[bpso, b_sm], w=[b_mixb])
                pst, bpst = bank()
                pst_bf = pst[:, :].bitcast(BF16)
                sc.group("pe", [I("transpose", pst_bf[:, m * 128:(m + 1) * 128],
                                  mixb[:, 2 * m:2 * m + 2, :].rearrange("p h d -> p (h d)"), ident_bf) for m in range(8)],
                         reads=[b_mixb, b_cst], writes=[bpst])
                ac(I("copy", out=aT[:, 4:12, tk], in_=pst_bf.rearrange("p (m t) -> p m t", t=128)), r=[bpst], w=b_a[4:12])
                ps, bps = bank()
                sc.group("pe", [I("matmul", ps[:, h * 128:(h + 1) * 128], mqk[:, 4 + h, tk], mqk[:, h, tk], start=True, stop=True)
                                for h in range(4)], reads=b_mqk, writes=[bps])
                for h in range(4):
                    dv(I("scalar_tensor_tensor", out=Sm[:, h, :], in0=ps[:, h * 128:(h + 1) * 128],
                         scalar=R(12, c * 4 + h, c * 4 + h + 1), in1=mle_bf, op0=ALU.mult, op1=ALU.mult),
                       r=[bps, b_sm, b_cst], w=[b_Sm])
                pn = [bank(), bank()]
                for hp in range(2):
                    psn, bpsn = pn[hp]
                    fns = []
                    for hh in range(2):
                        h = hp * 2 + hh
                        fns.append(I("matmul", psn[:, hh * 129:(hh + 1) * 129], Sm[:, h, :], mv[:, c, h, 0:129], start=True, stop=False))
                        fns.append(I("matmul", psn[:, hh * 129:(hh + 1) * 129], mqk[:, h, tk], Cbf[:, l, h, 0:129], start=False, stop=True))
                    sc.group("pe", fns, reads=[b_Sm, b_mv[c], b_Cbf[l]] + b_mqk, writes=[bpsn])
                for hp in range(2):
                    psn, bpsn = pn[hp]
                    pn3 = psn[:, 0:258].rearrange("p (h d) -> p h d", d=129)
                    dv(I("tensor_copy", out=R(18, hp * 2, hp * 2 + 2), in_=pn3[:, :, 128]), r=[bpsn, b_sm], w=smb)
                    for hh in range(2):
                        h = hp * 2 + hh
                        ac(I("activation", out=junk[:, 0:128], in_=psn[:, hh * 129:hh * 129 + 128], func=AF.Square,
                             accum_out=R(19, h, h + 1)), r=[bpsn, b_sm], w=[b_junk, b_sm])
                s1 = R(14, c * 4, c * 4 + 4)
                dv(I("tensor_tensor", out=R(20, 0, 4), in0=R(18, 0, 4), in1=s1, op=ALU.mult), r=smb, w=smb)
                dv(I("tensor_single_scalar", out=R(16, 4, 8), in_=R(20, 0, 4), scalar=-1.0, op=ALU.mult), r=smb, w=smb)
                dv(I("tensor_tensor", out=R(20, 0, 4), in0=R(20, 0, 4), in1=R(16, 4, 8), op=ALU.max), r=smb, w=smb)
                dv(I("tensor_single_scalar", out=R(20, 0, 4), in_=R(20, 0, 4), scalar=1.0, op=ALU.max), r=smb, w=smb)
                dv(I("reciprocal", out=R(21, 0, 4), in_=R(20, 0, 4)), r=smb, w=smb)
                dv(I("tensor_tensor", out=R(21, 0, 4), in0=R(21, 0, 4), in1=s1, op=ALU.mult), r=smb, w=smb)
                dv(I("tensor_tensor", out=R(22, 0, 4), in0=R(21, 0, 4), in1=R(21, 0, 4), op=ALU.mult), r=smb, w=smb)
                dv(I("tensor_tensor", out=R(22, 0, 4), in0=R(22, 0, 4), in1=R(19, 0, 4), op=ALU.mult), r=smb, w=smb)
                ac(I("activation", out=R(22, 0, 4), in_=R(22, 0, 4), func=AF.Sqrt, bias=eps_ap, scale=1.0 / 128), r=[b_sm, b_eps], w=smb)
                dv(I("reciprocal", out=R(23, 0, 4), in_=R(22, 0, 4)), r=smb, w=smb)
                dv(I("tensor_tensor", out=R(23, 0, 4), in0=R(23, 0, 4), in1=R(21, 0, 4), op=ALU.mult), r=smb, w=smb)
                for hp in range(2):
                    psn, bpsn = pn[hp]
                    for hh in range(2):
                        h = hp * 2 + hh
                        dv(I("scalar_tensor_tensor", out=mixf[:, h, :], in0=psn[:, hh * 129:hh * 129 + 128],
                             scalar=R(23, h, h + 1), in1=BC(l, 512 + h * 128, 512 + (h + 1) * 128), op0=ALU.mult, op1=ALU.mult),
                           r=[bpsn, b_sm, b_bcs], w=[b_mixf])
                dv(I("tensor_tensor", out=mixc[:, :, :], in0=mixf[:, :, :], in1=sgo[:, c, :].rearrange("p (h d) -> p h d", d=128),
                     op=ALU.mult), r=[b_mixf, b_sgo[c]], w=[b_mixc])
                pst, bpst = bank()
                pst_bf = pst[:, :].bitcast(BF16)
                sc.group("pe", [I("transpose", pst_bf[:, h * 128:(h + 1) * 128], mixc[:, h, :], ident_bf) for h in range(4)] +
                               [I("transpose", pst_bf[:, 512 + h * 128:512 + (h + 1) * 128], mqk[:, 4 + h, tk], ident_bf) for h in range(4)],
                         reads=[b_mixc, b_cst] + b_mqk, writes=[bpst])
                ac(I("copy", out=aT[:, 12:16, tk], in_=pst_bf[:, 0:512].rearrange("p (m t) -> p m t", t=128)), r=[bpst], w=b_a[12:16])
                dv(I("tensor_tensor", out=ktil[:, :, :], in0=pst_bf[:, 512:1024].rearrange("p (h d) -> p h d", d=128),
                     in1=R(13, c * 4, c * 4 + 4).unsqueeze(2).broadcast_to([128, 4, 128]), op=ALU.mult), r=[bpst, b_sm], w=[b_ktil])
                pu = [bank(), bank()]
                for hp in range(2):
                    psu, bpsu = pu[hp]
                    sc.group("pe", [I("matmul", psu[:, hh * 129:(hh + 1) * 129], ktil[:, hp * 2 + hh, :], mv[:, c, hp * 2 + hh, 0:129],
                                      start=True, stop=True) for hh in range(2)], reads=[b_ktil, b_mv[c]], writes=[bpsu])
                    for hh in range(2):
                        h = hp * 2 + hh
                        dv(I("scalar_tensor_tensor", out=C32[:, l, h, 0:129], in0=C32[:, l, h, 0:129],
                             scalar=R(15, c * 4 + h, c * 4 + h + 1), in1=psu[:, hh * 129:(hh + 1) * 129], op0=ALU.mult, op1=ALU.add),
                           r=[bpsu, b_sm, b_C32[l]], w=[b_C32[l]])
                ac(I("copy", out=Cbf[:, l, :, :], in_=C32[:, l, :, :]), r=[b_C32[l]], w=[b_Cbf[l]])

            if dbg == 7:
                sc.dma("sp", I("dma_start", out=dbg_d, in_=aT[:, :, :].rearrange("p c t -> p (c t)")), "setup", reads=b_a)
                return
            for o in range(KD):
                (s,) = take(1)
                ps, bps = fm_unit(s)
                dv(I("tensor_tensor", out=xT[:, o, :], in0=xT[:, o, :], in1=ps[:, :], op=ALU.add), r=[bps, b_x[o]], w=[b_x[o]])
            if dbg == 8:
                return
            rmsnorm_to_aT(l * 2 * KD + KD)
            adeps = {}
            for b in alias_small:
                _merge(adeps, b.w)
                _merge(adeps, b.r)
            for b in b_h:
                _merge(b.r, adeps)
            for f in range(KF):
                (sa,) = take(1)
                psg_, bpsg_ = fm_unit(sa)
                (sb_,) = take(1)
                psu_, bpsu_ = fm_unit(sb_)
                j = nxt("sg", 2)
                ac(I("activation", out=sg[:, j, :], in_=psg_[:, :], func=AF.Silu), r=[bpsg_], w=[b_sg[j]])
                dv(I("tensor_tensor", out=hT[:, f, :], in0=sg[:, j, :], in1=psu_[:, :], op=ALU.mult), r=[b_sg[j], bpsu_], w=[b_h[f]])
            for o in range(KD):
                slots = take(4)
                ps, bps = bank()
                fns = []
                for q in range(4):
                    for fl in range(11):
                        f = q * 11 + fl
                        fns.append(I("matmul", ps[:, :], wsl[:, slots[q], fl * 128:(fl + 1) * 128], hT[:, f, :],
                                     start=(f == 0), stop=(f == KF - 1)))
                sc.group("pe", fns, reads=b_h + [b_slot[s] for s in slots], writes=[bps])
                dv(I("tensor_tensor", out=xT[:, o, :], in0=xT[:, o, :], in1=ps[:, :], op=ALU.add), r=[bps, b_x[o]], w=[b_x[o]])

        for ti in range(NT):
            tok0 = ti * T
            sc.dma("sp", I("dma_start", out=xT[:, :, :], in_=x_d[ti].rearrange("p (c t) -> p c t", t=T)), "xld", writes=b_x)
            sc.dma("sp", I("dma_start", out=cs_t[:, 0, :], in_=cos_d[:, tok0:tok0 + T]), "cs", writes=[b_cs])
            sc.dma("sp", I("dma_start", out=cs_t[:, 1, :], in_=sin_d[:, tok0:tok0 + T]), "cs", writes=[b_cs])
            for l in range(L):
                layer_body(ti, l)
            ywr = [b_y] + b_h[0:32]
            if final_norm:
                norm_stats()
                for dch in range(KD):
                    gc = L * 2 * KD + dch
                    dv(I("scalar_tensor_tensor", out=yv3[:, dch, :], in0=xT[:, dch, :], scalar=g12[:, gc:gc + 1], in1=rs_b[:, :],
                         op0=ALU.mult, op1=ALU.mult), r=[b_x[dch], b_g12, b_rsb], w=ywr)
            else:
                for dch in range(KD):
                    ac(I("copy", out=yv3[:, dch, :], in_=xT[:, dch, :]), r=[b_x[dch]], w=ywr)
            sc.dma("sp", I("dma_start", out=y_d[ti], in_=yv), "yst", reads=ywr)
        sc.final_wait("sp", ywr)
        sc.final_wait("pool", [b_wbf])

        with nc.Block() as block:
            @block.tensor
            def _(e):
                sc.replay("pe", e)

            @block.scalar
            def _(e):
                sc.replay("act", e)

            @block.vector
            def _(e):
                sc.replay("dve", e)

            @block.gpsimd
            def _(e):
                sc.replay("pool", e)

            @block.sync
            def _(e):
                sc.replay("sp", e)
    return nc


def make_core_inputs(xb, layers, params, final_g, S):
    L = len(layers)
    NT = S // T
    xt = np.ascontiguousarray(xb.reshape(NT, T, KD, 128).transpose(0, 3, 2, 1)).reshape(NT, 128, KD * T)
    w32 = np.concatenate([_layer_units(params["w_in"][l], params["mlstm_conv_w"][l], params["w_out"][l],
                                       params["w_gate"][l], params["w_up"][l], params["w_down"][l]) for l in layers], 0)
    C, Sn = _rope_tables(S)
    g12 = np.zeros((128, L * 2 * KD + KD), np.float32)
    for i, l in enumerate(layers):
        g12[:, i * 32:i * 32 + 16] = params["norm1_g"][l].reshape(KD, 128).T
        g12[:, i * 32 + 16:i * 32 + 32] = params["norm2_g"][l].reshape(KD, 128).T
    g12[:, L * 32:] = final_g.reshape(KD, 128).T
    wst = np.zeros((128, L, 4, 128), np.float32)
    bc = np.zeros((1, L, 1568), np.float32)
    for i, l in enumerate(layers):
        wst[:, i] = params["gmlp_w_s"][l].transpose(2, 0, 1)
        bc[0, i, 0:512] = params["gmlp_v_gain"][l]
        bc[0, i, 512:1024] = params["mlstm_head_gain"][l]
        bc[0, i, 1024:1536] = params["gmlp_b_s"][l].reshape(-1)
        bc[0, i, 1536:1552] = params["attn_sinks"][l]
        bc[0, i, 1552:1556] = params["mlstm_i_bias"][l]
        bc[0, i, 1556:1560] = params["mlstm_f_bias"][l]
    return {"x": xt, "w32": w32, "cosT": C, "sinT": Sn, "consts": _consts().reshape(128, -1),
            "g12": g12, "wsT": wst.reshape(128, -1), "bcast": bc.reshape(1, -1)}


N_ACTIVE = 4


def kernel(**inputs):
    p = {k: np.asarray(v, dtype=np.float32) for k, v in inputs.items()}
    x = p["x"]
    Bn, S, _ = x.shape
    NT = S // T
    nc = build_program(DEPTH, NT, final_norm=True)
    base = make_core_inputs(x[0], list(range(DEPTH)), p, p["final_g"], S)
    in_maps = []
    for b in range(Bn):
        m = dict(base)
        if b > 0:
            m["x"] = np.ascontiguousarray(x[b].reshape(NT, T, KD, 128).transpose(0, 3, 2, 1)).reshape(NT, 128, KD * T)
        in_maps.append(m)
    res = run_bass_kernel_spmd(nc, in_maps, core_ids=list(range(Bn)))
    out = np.zeros((Bn, S, D), np.float32)
    for b in range(Bn):
        y = np.asarray(res.results[b]["y"]).reshape(NT, 128, KD, T)
        out[b] = y.transpose(0, 3, 2, 1).reshape(S, D)
    return out
```
